# Optimizing a Trainium2 kernel written in Bass

```python
import math
import jax, jax.numpy as jnp
from jax import lax
import numpy as np

D_MODEL = 1024
BATCH = 2
SEQ = 8192
DEPTH = 2

EPS = 1e-6
D_MIX = D_MODEL

POOL_WINDOWS = (2, 4, 8, 16)
POOL_GROUPS = 4
D_POOL = D_MIX // 4
POOL_GROUP_DIM = D_POOL // POOL_GROUPS

D_SSD = D_MIX // 2
SSD_HEAD_DIM = 64
SSD_HEADS = D_SSD // SSD_HEAD_DIM
SSD_GROUPS = 2
SSD_STATE = 128
SSD_CONV = 4
SSD_CHUNK = 128
D_SSD_XBC = D_SSD + 2 * SSD_GROUPS * SSD_STATE

D_MLSTM = D_MIX // 4
MLSTM_HEAD_DIM = 64
MLSTM_HEADS = D_MLSTM // MLSTM_HEAD_DIM
MLSTM_CONV = 4
MLSTM_CHUNK = 128

D_FF = 2816
FFN_CONV = 3

IN_SIZES = (D_POOL, D_SSD, D_SSD_XBC, SSD_HEADS, 2 * D_MLSTM, D_MLSTM, D_MLSTM, MLSTM_HEADS, MLSTM_HEADS)
IN_COLS = D_POOL + D_SSD + D_SSD_XBC + SSD_HEADS + 4 * D_MLSTM + 2 * MLSTM_HEADS

kernel_name = "hybrid_pool_ssd_mlstm_convffn"


def rmsnorm(x, w):
    xf = x.astype(jnp.float32)
    y = xf * lax.rsqrt(jnp.mean(xf * xf, axis=-1, keepdims=True) + EPS)
    return (y * w.astype(jnp.float32)).astype(x.dtype)


def causal_dwconv(x, w, b):
    K = w.shape[0]
    T = x.shape[1]
    xp = jnp.pad(x, ((0, 0), (K - 1, 0), (0, 0)))
    w = w.astype(x.dtype)
    y = xp[:, 0:T] * w[0]
    for k in range(1, K):
        y = y + xp[:, k:k + T] * w[k]
    return y + b.astype(x.dtype)


def split_cols(p, sizes):
    idx, acc = [], 0
    for s in sizes[:-1]:
        acc += s
        idx.append(acc)
    return jnp.split(p, idx, axis=-1)


def causal_mask(L):
    return jnp.tril(jnp.ones((L, L), dtype=bool))


def pool_mixer(u, w_pool, b_pool, pool_scale):
    Bsz, T, _ = u.shape
    uf = u.astype(jnp.float32).reshape(Bsz, T, POOL_GROUPS, POOL_GROUP_DIM)
    cs = jnp.pad(jnp.cumsum(uf, axis=1), ((0, 0), (1, 0), (0, 0), (0, 0)))
    pos = jnp.arange(1, T + 1, dtype=jnp.float32)
    means = []
    for g, win in enumerate(POOL_WINDOWS):
        c = cs[:, :, g]
        upper = c[:, 1:]
        lower = jnp.pad(c, ((0, 0), (win - 1, 0), (0, 0)))[:, :T]
        count = jnp.minimum(pos, float(win))
        means.append((upper - lower) / count[None, :, None])
    pooled = jnp.stack(means, axis=2) - uf
    y = jnp.einsum('btgc,gcd->btgd', pooled, w_pool.astype(jnp.float32))
    y = y + b_pool.astype(jnp.float32).reshape(POOL_GROUPS, POOL_GROUP_DIM)
    y = y.reshape(Bsz, T, D_POOL) * pool_scale.astype(jnp.float32)
    return y.astype(u.dtype)


def segsum(a):
    cs = jnp.cumsum(a, axis=-1)
    seg = cs[..., :, None] - cs[..., None, :]
    return jnp.where(causal_mask(a.shape[-1]), seg, -jnp.inf)


def ssd_mixer(z, xbc, dt_raw, conv_w, conv_b, dt_bias, a_log, d_skip, norm_w):
    Bsz, T, _ = z.shape
    H, P, G, N, L = SSD_HEADS, SSD_HEAD_DIM, SSD_GROUPS, SSD_STATE, SSD_CHUNK
    nc = T // L
    xbc = jax.nn.silu(causal_dwconv(xbc, conv_w, conv_b))
    xs, Bm, Cm = jnp.split(xbc, [D_SSD, D_SSD + G * N], axis=-1)
    dt = jax.nn.softplus(dt_raw.astype(jnp.float32) + dt_bias.astype(jnp.float32))
    A = -jnp.exp(a_log.astype(jnp.float32))
    x = xs.astype(jnp.float32).reshape(Bsz, T, H, P)
    Bh = jnp.repeat(Bm.astype(jnp.float32).reshape(Bsz, T, G, N), H // G, axis=2)
    Ch = jnp.repeat(Cm.astype(jnp.float32).reshape(Bsz, T, G, N), H // G, axis=2)
    X = (x * dt[..., None]).reshape(Bsz, nc, L, H, P)
    Bc = Bh.reshape(Bsz, nc, L, H, N)
    Cc = Ch.reshape(Bsz, nc, L, H, N)
    Adt = (dt * A).reshape(Bsz, nc, L, H).transpose(0, 3, 1, 2)
    A_cs = jnp.cumsum(Adt, axis=-1)
    Lmat = jnp.exp(segsum(Adt))
    y_diag = jnp.einsum('bclhn,bcshn,bhcls,bcshp->bclhp', Cc, Bc, Lmat, X)
    decay = jnp.exp(A_cs[..., -1:] - A_cs)
    states = jnp.einsum('bclhn,bhcl,bclhp->bchpn', Bc, decay, X)
    chunk_decay = jnp.exp(A_cs[..., -1])

    def step(carry, inp):
        dec, st = inp
        return carry * dec[..., None, None] + st, carry

    init = jnp.zeros((Bsz, H, P, N), jnp.float32)
    _, prev = lax.scan(step, init, (chunk_decay.transpose(2, 0, 1), states.transpose(1, 0, 2, 3, 4)))
    prev = prev.transpose(1, 0, 2, 3, 4)
    y_off = jnp.einsum('bclhn,bchpn,bhcl->bclhp', Cc, prev, jnp.exp(A_cs))
    y = (y_diag + y_off).reshape(Bsz, T, H, P) + x * d_skip.astype(jnp.float32)[:, None]
    y = y.reshape(Bsz, T, D_SSD) * jax.nn.silu(z.astype(jnp.float32))
    return rmsnorm(y, norm_w).astype(z.dtype)


def mlstm_mixer(qk, v, o_raw, i_raw, f_raw, conv_w, conv_b, i_bias, f_bias, norm_w):
    Bsz, T, _ = v.shape
    H, Dh, L = MLSTM_HEADS, MLSTM_HEAD_DIM, MLSTM_CHUNK
    nc = T // L
    qk = jax.nn.silu(causal_dwconv(qk, conv_w, conv_b))
    q, k = jnp.split(qk, 2, axis=-1)
    q = q.astype(jnp.float32).reshape(Bsz, nc, L, H, Dh) * (Dh ** -0.5)
    k = k.astype(jnp.float32).reshape(Bsz, nc, L, H, Dh)
    vv = v.astype(jnp.float32).reshape(Bsz, nc, L, H, Dh)
    log_i = (i_raw.astype(jnp.float32) + i_bias.astype(jnp.float32))
    log_f = jax.nn.log_sigmoid(f_raw.astype(jnp.float32) + f_bias.astype(jnp.float32))
    log_i = log_i.reshape(Bsz, nc, L, H).transpose(0, 3, 1, 2)
    log_f = log_f.reshape(Bsz, nc, L, H).transpose(0, 3, 1, 2)
    b = jnp.cumsum(log_f, axis=-1)
    Dlog = b[..., :, None] - b[..., None, :] + log_i[..., None, :]
    Dlog = jnp.where(causal_mask(L), Dlog, -jnp.inf)
    a = b[..., -1:] - b + log_i
    m_loc = jnp.max(a, axis=-1)
    w_loc = jnp.exp(a - m_loc[..., None])
    C_loc = jnp.einsum('bhcl,bclhd,bclhe->bchde', w_loc, k, vv)
    n_loc = jnp.einsum('bhcl,bclhd->bchd', w_loc, k)
    b_last = b[..., -1]

    def step(carry, inp):
        C, n, m = carry
        bl, ml, Cl, nl = inp
        m_new = jnp.maximum(bl + m, ml)
        sp = jnp.exp(bl + m - m_new)
        sl = jnp.exp(ml - m_new)
        C_new = sp[..., None, None] * C + sl[..., None, None] * Cl
        n_new = sp[..., None] * n + sl[..., None] * nl
        return (C_new, n_new, m_new), (C, n, m)

    init = (jnp.zeros((Bsz, H, Dh, Dh), jnp.float32), jnp.zeros((Bsz, H, Dh), jnp.float32),
            jnp.zeros((Bsz, H), jnp.float32))
    xs_in = (b_last.transpose(2, 0, 1), m_loc.transpose(2, 0, 1),
             C_loc.transpose(1, 0, 2, 3, 4), n_loc.transpose(1, 0, 2, 3))
    _, (prev_C, prev_n, prev_m) = lax.scan(step, init, xs_in)
    prev_C = prev_C.transpose(1, 0, 2, 3, 4)
    prev_n = prev_n.transpose(1, 0, 2, 3)
    prev_m = prev_m.transpose(1, 2, 0)
    inter_log = b + prev_m[..., None]
    m_t = jnp.maximum(inter_log, jnp.max(Dlog, axis=-1))
    w_intra = jnp.exp(Dlog - m_t[..., None])
    w_inter = jnp.exp(inter_log - m_t)
    s = jnp.einsum('bclhd,bcshd->bhcls', q, k) * w_intra
    num = (jnp.einsum('bhcls,bcshe->bclhe', s, vv)
           + jnp.einsum('bclhd,bchde,bhcl->bclhe', q, prev_C, w_inter))
    den = jnp.sum(s, axis=-1) + jnp.einsum('bclhd,bchd,bhcl->bhcl', q, prev_n, w_inter)
    den = jnp.maximum(jnp.abs(den), jnp.exp(-m_t)).transpose(0, 2, 3, 1)
    h = (num / den[..., None]).reshape(Bsz, T, H, Dh)
    h = jax.nn.sigmoid(o_raw.astype(jnp.float32)).reshape(Bsz, T, H, Dh) * h
    h = h * lax.rsqrt(jnp.mean(h * h, axis=-1, keepdims=True) + EPS)
    h = h.reshape(Bsz, T, D_MLSTM) * norm_w.astype(jnp.float32)
    return h.astype(v.dtype)


def conv_ffn(h, w_up, conv_w, conv_b, w_down):
    u = causal_dwconv(h @ w_up, conv_w, conv_b)
    g, val = jnp.split(u, 2, axis=-1)
    return (jax.nn.gelu(g, approximate=True) * val) @ w_down


def setup_inputs(seed: int = 0) -> dict:
    key = jax.random.key(seed)
    ks = jax.random.split(key, 32)
    f32 = jnp.float32

    def nrm(k, shape, scale):
        return jax.random.normal(k, shape, f32) * scale

    def gain(k, shape):
        return 1.0 + 0.02 * jax.random.normal(k, shape, f32)

    dt0 = jnp.exp(jax.random.uniform(ks[9], (DEPTH, SSD_HEADS), f32, math.log(1e-3), math.log(1e-1)))
    dt_bias = dt0 + jnp.log(-jnp.expm1(-dt0))
    f_bias = jnp.linspace(3.0, 6.0, MLSTM_HEADS, dtype=f32)[None, :] + 0.1 * jax.random.normal(ks[16], (DEPTH, MLSTM_HEADS), f32)
    return {
        "x": jax.random.normal(ks[0], (BATCH, SEQ, D_MODEL), f32),
        "pre_mix_norm": gain(ks[1], (DEPTH, D_MODEL)),
        "w_in": nrm(ks[2], (DEPTH, D_MODEL, IN_COLS), D_MODEL ** -0.5),
        "pool_w": nrm(ks[3], (DEPTH, POOL_GROUPS, POOL_GROUP_DIM, POOL_GROUP_DIM), POOL_GROUP_DIM ** -0.5),
        "pool_b": nrm(ks[4], (DEPTH, D_POOL), 0.02),
        "pool_scale": gain(ks[5], (DEPTH, D_POOL)),
        "ssd_conv_w": nrm(ks[6], (DEPTH, SSD_CONV, D_SSD_XBC), SSD_CONV ** -0.5),
        "ssd_conv_b": nrm(ks[7], (DEPTH, D_SSD_XBC), 0.02),
        "ssd_dt_bias": dt_bias,
        "ssd_a_log": jnp.log(jax.random.uniform(ks[10], (DEPTH, SSD_HEADS), f32, 1.0, 16.0)),
        "ssd_d": gain(ks[11], (DEPTH, SSD_HEADS)),
        "ssd_norm": gain(ks[12], (DEPTH, D_SSD)),
        "mlstm_conv_w": nrm(ks[13], (DEPTH, MLSTM_CONV, 2 * D_MLSTM), MLSTM_CONV ** -0.5),
        "mlstm_conv_b": nrm(ks[14], (DEPTH, 2 * D_MLSTM), 0.02),
        "mlstm_i_bias": nrm(ks[15], (DEPTH, MLSTM_HEADS), 0.1),
        "mlstm_f_bias": f_bias,
        "mlstm_norm": gain(ks[17], (DEPTH, D_MLSTM)),
        "w_out": nrm(ks[18], (DEPTH, D_MIX, D_MODEL), D_MIX ** -0.5),
        "post_mix_norm": gain(ks[19], (DEPTH, D_MODEL)),
        "pre_ffn_norm": gain(ks[20], (DEPTH, D_MODEL)),
        "ffn_w_up": nrm(ks[21], (DEPTH, D_MODEL, 2 * D_FF), D_MODEL ** -0.5),
        "ffn_conv_w": nrm(ks[22], (DEPTH, FFN_CONV, 2 * D_FF), FFN_CONV ** -0.5),
        "ffn_conv_b": nrm(ks[23], (DEPTH, 2 * D_FF), 0.02),
        "ffn_w_down": nrm(ks[24], (DEPTH, D_FF, D_MODEL), D_FF ** -0.5),
        "post_ffn_norm": gain(ks[25], (DEPTH, D_MODEL)),
    }


def reference(x, pre_mix_norm, w_in, pool_w, pool_b, pool_scale, ssd_conv_w, ssd_conv_b,
              ssd_dt_bias, ssd_a_log, ssd_d, ssd_norm, mlstm_conv_w, mlstm_conv_b,
              mlstm_i_bias, mlstm_f_bias, mlstm_norm, w_out, post_mix_norm, pre_ffn_norm,
              ffn_w_up, ffn_conv_w, ffn_conv_b, ffn_w_down, post_ffn_norm):
    for l in range(DEPTH):
        h = rmsnorm(x, pre_mix_norm[l])
        p = h @ w_in[l]
        u_pool, z, xbc, dt_raw, qk, v, o_raw, i_raw, f_raw = split_cols(p, IN_SIZES)
        y_pool = pool_mixer(u_pool, pool_w[l], pool_b[l], pool_scale[l])
        y_ssd = ssd_mixer(z, xbc, dt_raw, ssd_conv_w[l], ssd_conv_b[l], ssd_dt_bias[l],
                          ssd_a_log[l], ssd_d[l], ssd_norm[l])
        y_mlstm = mlstm_mixer(qk, v, o_raw, i_raw, f_raw, mlstm_conv_w[l], mlstm_conv_b[l],
                              mlstm_i_bias[l], mlstm_f_bias[l], mlstm_norm[l])
        mix = jnp.concatenate([y_pool, y_ssd, y_mlstm], axis=-1) @ w_out[l]
        x = x + rmsnorm(mix, post_mix_norm[l])
        h = rmsnorm(x, pre_ffn_norm[l])
        f = conv_ffn(h, ffn_w_up[l], ffn_conv_w[l], ffn_conv_b[l], ffn_w_down[l])
        x = x + rmsnorm(f, post_ffn_norm[l])
    return x
```

```python
import math
from contextlib import ExitStack

import numpy as np
import concourse.bass as bass
import concourse.mybir as mybir
from concourse.bass_utils import run_bass_kernel_spmd

F32 = mybir.dt.float32
BF = mybir.dt.bfloat16
AF = mybir.ActivationFunctionType
ALU = mybir.AluOpType

NCORES = 8
D = 1024
KT = 8
T = 2048
HALO = 16
TT = T + HALO
NCH = 16
DEPTH = 2
DFF = 2816
NJ = 22
EPS = 1e-6
NPP = 284
NPB = 24
NCONST = 640
NCM = 56
AGW = 784
LN8 = math.log(0.125)

PP_G1, PP_PMN, PP_G2, PP_PFN = 0, 8, 16, 24
PP_POOLB, PP_POOLS, PP_INVW = 32, 34, 36
PP_CXBC, PP_CQK = 38, 78
PP_SSDD, PP_SSDN, PP_MLN = 98, 102, 106
PP_FFN = 108
C_U, C_MASK, C_ID, C_ONE, C_OBLK = 0, 128, 256, 384, 512
CM_SEL, CM_PREC, CM_NPREC, CM_INVC = 0, 8, 16, 24


class Tl:
    def __init__(self, h, shape):
        self.h = h
        self.shape = list(shape)
        self.pitch = int(np.prod(shape[1:]))

    def a(self, off=0, pat=None, p0=0, pn=None):
        if pn is None:
            pn = self.shape[0] - p0
        if pat is None:
            pat = [[1, self.pitch]]
        return bass.AP(self.h, p0 * self.pitch + off, [[self.pitch, pn]] + [list(x) for x in pat])


class Prog:
    def __init__(self, debug=None, needed=None):
        self.nc = bass.Bass("TRN2", target_bir_lowering=False)
        self.es = ExitStack()
        self.debug = debug or []
        self.dbg_out = {}
        nc = self.nc
        self.eng = {"pe": nc.tensor, "act": nc.scalar, "dve": nc.vector, "pool": nc.gpsimd, "sp": nc.sync}
        self.esem = {}
        self.ecnt = {}
        for e in ["pe", "act", "dve", "pool"]:
            self.esem[e] = self.es.enter_context(nc.semaphore("c_" + e))
            self.ecnt[e] = 0
        self.dsem = {}
        self.waited = {e: {} for e in self.eng}
        self.state = {}
        self.needed = needed
        self.rec = {e: set() for e in self.esem}
        if needed is not None:
            self.rank = {e: {sq: i + 1 for i, sq in enumerate(sorted(needed[e]))} for e in needed}

    def _deps(self, reads, writes):
        deps = []
        for r in reads:
            st = self.state.get(r)
            if st and st["w"]:
                deps.append(st["w"])
        for w in writes:
            st = self.state.get(w)
            if st:
                if st["w"]:
                    deps.append(st["w"])
                deps.extend(st["r"].values())
        return deps

    def _wait(self, e, deps):
        for (name, sem, val) in deps:
            if name == "c_pe" and e == "pe":
                continue
            if self.waited[e].get(name, 0) >= val:
                continue
            self.waited[e][name] = val
            if name.startswith("c_"):
                pe_ = name[2:]
                self.rec[pe_].add(val)
                if self.needed is None:
                    continue
                self.eng[e].wait_ge(sem, self.rank[pe_][val])
            else:
                if self.needed is None:
                    continue
                self.eng[e].wait_ge(sem, val)

    def _commit(self, dep, reads, writes):
        for w in writes:
            self.state[w] = {"w": dep, "r": {}}
        for r in reads:
            if r in writes:
                continue
            st = self.state.setdefault(r, {"w": None, "r": {}})
            st["r"][dep[0]] = dep

    def op(self, e, reads, writes, fn):
        self._wait(e, self._deps(reads, writes))
        self.ecnt[e] += 1
        seq = self.ecnt[e]
        if self.needed is not None:
            ins = fn(self.eng[e])
            if seq in self.rank[e]:
                ins.then_inc(self.esem[e], 1)
        dep = ("c_" + e, self.esem[e], seq)
        self._commit(dep, reads, writes)

    def _dsem(self, key):
        if key not in self.dsem:
            nm = "d%d" % len(self.dsem)
            self.dsem[key] = [nm, self.es.enter_context(self.nc.semaphore(nm)), 0]
        return self.dsem[key]

    def dma(self, q, out, in_, reads, writes, semkey=None):
        if semkey is None:
            semkey = writes[0]
        self._wait(q, self._deps(reads, writes))
        d = self._dsem(semkey)
        d[2] += 16
        if self.needed is not None:
            ins = self.eng[q].dma_start(out=out, in_=in_)
            ins.then_inc(d[1], 16)
        dep = (d[0], d[1], d[2])
        self._commit(dep, reads, writes)

    def allgather(self, in_t, out_t, reads, writes, semkey):
        q = "pool"
        if getattr(self, "sim", False) == "ext":
            self.op("pool", reads, writes, lambda e: e.memset(self.simscr.a(), 0.0))
            return
        if getattr(self, "sim", False):
            w = in_t.shape[1]
            for r in range(NCORES):
                self.dma("pool", out_t.a(0, [[1, w]], p0=r * 128, pn=128), in_t.a(), reads, [(writes[0], r)], semkey=(semkey, r))
            self.op("pool", [(writes[0], r) for r in range(NCORES)], writes, lambda e: e.memset(self.simscr.a(), 0.0))
            return
        self._wait(q, self._deps(reads, writes))
        d = self._dsem(semkey)
        d[2] += 1
        if self.needed is not None:
            ins = self.nc.gpsimd.collective_compute(
                "AllGather", ALU.bypass, replica_groups=[list(range(NCORES))],
                ins=[in_t.h.ap().opt()], outs=[out_t.h.ap().opt()])
            ins.then_inc(d[1], 1)
        dep = (d[0], d[1], d[2])
        self._commit(dep, reads, writes)

    def barrier(self):
        targets = [("c_" + e, self.esem[e], self.ecnt[e]) for e in self.esem if self.ecnt[e] > 0]
        targets += [(d[0], d[1], d[2]) for d in self.dsem.values() if d[2] > 0]
        for e in self.eng:
            self._wait(e, targets)
        self.state = {}

    def sb(self, name, shape, dt):
        self.nalloc = getattr(self, "nalloc", 0) + 1
        cm = self.nc.sbuf_tensor("s%d_%s" % (self.nalloc, name), list(shape), dt)
        h = cm.__enter__()
        return Tl(h, shape), cm

    def free(self, cms):
        for cm in reversed(cms):
            cm.__exit__(None, None, None)

    def dram(self, name, shape, dt, kind=None):
        if kind is None:
            h = self.nc.dram_tensor(name, list(shape), dt)
        else:
            h = self.nc.dram_tensor(name, list(shape), dt, kind=kind)
        return Tl(h, shape)


def build_program(L, debug=None, stop_after=None, sim=False, dbg_skip_p1=False, dbg_nch=NCH):
    dry = _build(L, debug, stop_after, sim, dbg_skip_p1, dbg_nch, None)
    return _build(L, debug, stop_after, sim, dbg_skip_p1, dbg_nch, dry.rec)


def _build(L, debug, stop_after, sim, dbg_skip_p1, dbg_nch, needed):
    P = Prog(debug=debug, needed=needed)
    nc = P.nc
    P.sim = sim
    if sim:
        P.simscr, _ = P.sb("simscr", [128, 4], F32)

    xT = P.dram("xT", [128, KT, TT], F32, "ExternalInput")
    wm = P.dram("wm", [L * 128, KT, 2560], F32, "ExternalInput")
    wvg = P.dram("wvg", [L * 128, KT, 272], F32, "ExternalInput")
    wo = P.dram("wo", [L * 128, KT, 1024], F32, "ExternalInput")
    wu = P.dram("wu", [L * 128, KT, 2 * DFF], F32, "ExternalInput")
    wd = P.dram("wd", [L * 128, NJ, 1024], F32, "ExternalInput")
    pw = P.dram("pw", [L * 128, 2, 128], F32, "ExternalInput")
    pp_d = P.dram("pp", [L * 128, NPP], F32, "ExternalInput")
    pb_d = P.dram("pb", [L * 128, NPB], F32, "ExternalInput")
    consts_d = P.dram("consts", [128, NCONST], F32, "ExternalInput")
    cm_d = P.dram("cm", [128, NCM], F32, "ExternalInput")
    out_d = P.dram("out", [128, KT, T], F32, "ExternalOutput")
    xres = P.dram("xres", [128, KT, TT], F32)
    n_ag = 3 * L
    cc_in = [P.dram("cc_in%d" % i, [128, AGW if i % 3 == 0 else 128], F32) for i in range(n_ag)]
    cc_out = [P.dram("cc_out%d" % i, [NCORES * 128, AGW if i % 3 == 0 else 128], F32) for i in range(n_ag)]

    consts, _ = P.sb("consts", [128, NCONST], F32)
    cmt, _ = P.sb("cmt", [128, NCM], F32)
    ident_bf, _ = P.sb("ident_bf", [128, 128], BF)
    ones_bf, _ = P.sb("ones_bf", [128, 128], BF)
    onesblk_bf, _ = P.sb("onesblk_bf", [128, 128], BF)
    U_bf, _ = P.sb("U_bf", [128, 128], BF)
    ppt, _ = P.sb("ppt", [128, L, NPP], F32)
    pbt, _ = P.sb("pbt", [128, L, NPB], F32)
    epst, _ = P.sb("epst", [128, 4], F32)

    ps = []
    for i in range(7):
        cmgr = nc.psum_tensor("ps%d" % i, [128, 512], F32)
        ps.append(Tl(cmgr.__enter__(), [128, 512]))
    cmgr = nc.psum_tensor("ps7", [128, 512], F32)
    ps.append(Tl(cmgr.__enter__(), [128, 512]))

    P.dma("sp", consts.a(), consts_d.a(), ["consts_d"], ["consts"])
    P.dma("sp", cmt.a(), cm_d.a(), ["cm_d"], ["cmt"])
    for l in range(L):
        P.dma("sp", ppt.a(l * NPP, [[1, NPP]]), pp_d.a(0, [[1, NPP]], p0=l * 128, pn=128), ["pp_d"], [("ppt", l)])
        P.dma("sp", pbt.a(l * NPB, [[1, NPB]]), pb_d.a(0, [[1, NPB]], p0=l * 128, pn=128), ["pb_d"], [("pbt", l)])
    P.op("dve", ["consts"], ["ident_bf"], lambda e: e.tensor_copy(out=ident_bf.a(), in_=consts.a(C_ID, [[1, 128]])))
    P.op("dve", ["consts"], ["ones_bf"], lambda e: e.tensor_copy(out=ones_bf.a(), in_=consts.a(C_ONE, [[1, 128]])))
    P.op("dve", ["consts"], ["onesblk_bf"], lambda e: e.tensor_copy(out=onesblk_bf.a(), in_=consts.a(C_OBLK, [[1, 128]])))
    P.op("dve", ["consts"], ["U_bf"], lambda e: e.tensor_copy(out=U_bf.a(), in_=consts.a(C_U, [[1, 128]])))
    P.op("dve", [], ["epst"], lambda e: e.memset(epst.a(0, [[1, 1]]), EPS))
    P.op("dve", ["epst"], ["epst"], lambda e: e.memset(epst.a(1, [[1, 1]]), 1.0))
    P.op("dve", ["epst"], ["epst"], lambda e: e.memset(epst.a(2, [[1, 1]]), LN8))
    P.op("dve", ["epst"], ["epst"], lambda e: e.memset(epst.a(3, [[1, 1]]), 0.0))
    P.barrier()

    eps_ap = lambda: epst.a(0, [[1, 1]])
    one_ap = lambda: epst.a(1, [[1, 1]])
    ln8_ap = lambda: epst.a(2, [[1, 1]])

    def pp(l, col, n=1):
        return ppt.a(l * NPP + col, [[1, n]])

    def cmc(col, n=1):
        return cmt.a(col, [[1, n]])

    blocks5 = [(0, 416), (416, 416), (832, 416), (1248, 416), (1664, 400)]
    blocks4 = [(HALO + 512 * i, 512) for i in range(4)]

    def dbg_dump(name, tl, dt):
        if name not in P.debug:
            return
        d = P.dram("dbg_" + name, tl.shape, dt, "ExternalOutput")
        P.barrier()
        P.dma("sp", d.a(), tl.a(), [], [("dbg", name)])
        P.barrier()
        P.dbg_out[name] = d

    def rms_rstd(psum_ap, count, rs_out, tmp_out, pskey, rs_key, tmp_key):
        P.op("act", [pskey, "epst"], [tmp_key],
             lambda e: e.activation(out=tmp_out, in_=psum_ap, func=AF.Ln, bias=eps_ap(), scale=1.0 / count))
        P.op("act", [tmp_key], [rs_key], lambda e: e.activation(out=rs_out, in_=tmp_out, func=AF.Exp, scale=-0.5))

    def norm_phase(l, src, srckey, gcol, hT, pfx):
        xblk, c1 = P.sb(pfx + "xblk", [128, 2, KT, 512], F32)
        sqb, c2 = P.sb(pfx + "sqb", [128, 2, KT, 512], BF)
        lnt, c3 = P.sb(pfx + "lnt", [128, 2, 512], F32)
        rst, c4 = P.sb(pfx + "rst", [128, 2, 512], F32)
        for bi, (c0, n) in enumerate(blocks5):
            s = bi % 2
            xo = s * KT * 512
            P.dma("sp", xblk.a(xo, [[512, KT], [1, n]]), src.a(c0, [[TT, KT], [1, n]]), [srckey], [("xblk", s)])
            P.op("act", [("xblk", s)], [("sqb", s)],
                 lambda e: e.activation(out=sqb.a(xo, [[512, KT], [1, n]]), in_=xblk.a(xo, [[512, KT], [1, n]]),
                                        func=AF.Square))
            for k in range(KT):
                P.op("pe", [("sqb", s), "ones_bf"], [("ps", s)],
                     lambda e: e.matmul(ps[s].a(0, [[1, n]]), lhsT=ones_bf.a(), rhs=sqb.a(xo + k * 512, [[1, n]]),
                                        start=(k == 0), stop=(k == KT - 1)))
            rms_rstd(ps[s].a(0, [[1, n]]), float(D), rst.a(s * 512, [[1, n]]), lnt.a(s * 512, [[1, n]]),
                     ("ps", s), ("rst", s), ("lnt", s))
            for k in range(KT):
                P.op("dve", [("xblk", s), ("rst", s), ("ppt", l)], ["hT"],
                     lambda e: e.scalar_tensor_tensor(out=hT.a(k * TT + c0, [[1, n]]), in0=xblk.a(xo + k * 512, [[1, n]]),
                                                      scalar=pp(l, gcol + k), in1=rst.a(s * 512, [[1, n]]),
                                                      op0=ALU.mult, op1=ALU.mult))
        P.barrier()
        P.free([c1, c2, c3, c4])

    def halo_exchange(ag_idx, payload_tl, payload_key, dest_dram, pfx):
        agin, c1 = P.sb(pfx + "agin", [128, NCORES, 128], F32)
        hal, c2 = P.sb(pfx + "hal", [128, 128], F32)
        P.dma("pool", cc_in[ag_idx].a(), payload_tl.a(), [payload_key], [("ccin", ag_idx)])
        P.allgather(cc_in[ag_idx], cc_out[ag_idx], [("ccin", ag_idx)], [("ccout", ag_idx)], ("ag", ag_idx))
        P.dma("pool", agin.a(0, [[128, NCORES], [1, 128]]),
              bass.AP(cc_out[ag_idx].h, 0, [[128, 128], [128 * 128, NCORES], [1, 128]]),
              [("ccout", ag_idx)], [pfx + "agin"])
        for r in range(NCORES):
            if r == 0:
                P.op("dve", [pfx + "agin", "cmt"], [pfx + "hal"],
                     lambda e: e.tensor_scalar(out=hal.a(), in0=agin.a(0, [[1, 128]]), scalar1=cmc(CM_SEL + 0),
                                               scalar2=None, op0=ALU.mult))
            else:
                P.op("dve", [pfx + "agin", "cmt", pfx + "hal"], [pfx + "hal"],
                     lambda e: e.scalar_tensor_tensor(out=hal.a(), in0=agin.a(r * 128, [[1, 128]]),
                                                      scalar=cmc(CM_SEL + r), in1=hal.a(), op0=ALU.mult, op1=ALU.add))
        P.dma("sp", dest_dram.a(0, [[TT, KT], [1, HALO]]), hal.a(0, [[HALO, KT], [1, HALO]]), [pfx + "hal"], ["xres_halo"])
        P.barrier()
        P.free([c1, c2])

    def out_proj_phase(l, nk, lhs_fn, rhs_fn, rkeys, gcol, xsrc, xsrc_off, xdst, xdst_tt, xdst_off, pfx, last16=None):
        xb, c1 = P.sb(pfx + "xb", [128, 2, KT, 256], F32)
        sq, c2 = P.sb(pfx + "sq", [128, KT, 256], BF)
        lnt, c3 = P.sb(pfx + "lnt", [128, 256], F32)
        rst, c4 = P.sb(pfx + "rst", [128, 256], F32)
        tmp, c5 = P.sb(pfx + "tmp", [128, 2, 256], F32)
        for b in range(8):
            s = b % 2
            xo = s * KT * 256
            t0 = b * 256
            P.dma("sp", xb.a(xo, [[256, KT], [1, 256]]), xsrc.a(xsrc_off + t0, [[TT, KT], [1, 256]]),
                  ["xres_all"], [("xb", s)])
            for m in range(KT):
                bank = m // 2
                co = (m % 2) * 256
                for k in range(nk):
                    P.op("pe", rkeys, [("ps", bank)],
                         lambda e: e.matmul(ps[bank].a(co, [[1, 256]]), lhsT=lhs_fn(k, m), rhs=rhs_fn(k, t0),
                                            start=(k == 0), stop=(k == nk - 1)))
            for bank in range(4):
                P.op("act", [("ps", bank)], [pfx + "sq"],
                     lambda e: e.activation(out=sq.a(bank * 512, [[1, 512]]), in_=ps[bank].a(), func=AF.Square))
            for m in range(KT):
                P.op("pe", [pfx + "sq", "ones_bf"], [("ps", 4)],
                     lambda e: e.matmul(ps[4].a(0, [[1, 256]]), lhsT=ones_bf.a(), rhs=sq.a(m * 256, [[1, 256]]),
                                        start=(m == 0), stop=(m == KT - 1)))
            rms_rstd(ps[4].a(0, [[1, 256]]), float(D), rst.a(), lnt.a(), ("ps", 4), pfx + "rst", pfx + "lnt")
            for m in range(KT):
                bank = m // 2
                co = (m % 2) * 256
                ts = m % 2
                P.op("dve", [("ps", bank), pfx + "rst", ("ppt", l)], [(pfx + "tmp", ts)],
                     lambda e: e.scalar_tensor_tensor(out=tmp.a(ts * 256, [[1, 256]]), in0=ps[bank].a(co, [[1, 256]]),
                                                      scalar=pp(l, gcol + m), in1=rst.a(), op0=ALU.mult, op1=ALU.mult))
                P.op("dve", [(pfx + "tmp", ts), ("xb", s)], [("xb", s)],
                     lambda e: e.tensor_tensor(out=xb.a(xo + m * 256, [[1, 256]]), in0=xb.a(xo + m * 256, [[1, 256]]),
                                               in1=tmp.a(ts * 256, [[1, 256]]), op=ALU.add))
            if last16 is not None and b == 7:
                P.op("act", [("xb", s)], ["last16"],
                     lambda e: e.activation(out=last16.a(0, [[HALO, KT], [1, HALO]]),
                                            in_=xb.a(xo + 256 - HALO, [[256, KT], [1, HALO]]), func=AF.Copy))
            P.dma("sp", xdst.a(xdst_off + t0, [[xdst_tt, KT], [1, 256]]), xb.a(xo, [[256, KT], [1, 256]]),
                  [("xb", s)], [("xdst", b)], semkey=("xst", s))
        P.barrier()
        P.free([c1, c2, c3, c4, c5])

    for l in range(L):
        src = xT if l == 0 else xres
        last = (l == L - 1)
        agb = 3 * l
        zs, m1 = P.sb("zs", [128, 4, T], BF)
        xs, m2 = P.sb("xs", [128, 4, T], BF)
        BC, m3 = P.sb("BC", [128, 4, T], BF)
        qk, m4 = P.sb("qk", [128, 4, T], BF)
        so, m5 = P.sb("so", [128, 2, T], BF)
        vtok, m6 = P.sb("vtok", [128, NCH, 256], BF)
        ypool, m7 = P.sb("ypool", [128, 2, T], BF)
        graw, m8 = P.sb("graw", [128, NCH, 16], F32)
        mixer_cms = [m1, m2, m3, m4, m5, m6, m7, m8]

        hT, t1 = P.sb("hT", [128, KT, TT], BF)
        norm_phase(l, src, "xsrc_all", PP_G1, hT, "n1_")
        if l == 0:
            dbg_dump("hT", hT, BF)

        pwb, t8 = P.sb("pwb", [128, 2, 128], BF)
        ut, t2 = P.sb("ut", [128, 2, TT], F32)
        cpreA, t3 = P.sb("cpreA", [128, TT], F32)
        cpreB, t4 = P.sb("cpreB", [128, TT], F32)
        ctmp, t5 = P.sb("ctmp", [128, 2, T], F32)
        wb, t6 = P.sb("wb", [128, 2, KT, 512], BF)
        wvgb, t7 = P.sb("wvgb", [128, KT, 272], BF)

        P.dma("pool", wvgb.a(), wvg.a(0, [[1, KT * 272]], p0=l * 128, pn=128), ["wvg_d"], ["wvgb"])
        P.dma("pool", pwb.a(), pw.a(0, [[1, 256]], p0=l * 128, pn=128), ["pw_d"], ["pwb"])

        psrot = [0]
        cpre_slot = [0]
        for gi in range(0 if dbg_skip_p1 else 5):
            ws = gi % 2
            wo_ = ws * KT * 512
            P.dma("pool", wb.a(wo_, [[512, KT], [1, 512]]),
                  wm.a(gi * 512, [[2560, KT], [1, 512]], p0=l * 128, pn=128), ["wm_d"], [("wb", ws)])
            for ti in range(4):
                ct = gi * 4 + ti
                need_halo = ct in (0, 1) or 6 <= ct <= 17
                is_conv = 6 <= ct <= 17
                blks = blocks5 if need_halo else blocks4
                if is_conv:
                    cs_ = cpre_slot[0] % 2
                    cpre_slot[0] += 1
                    cpre = cpreA if cs_ == 0 else cpreB
                for (c0, n) in blks:
                    pi = 2 + (psrot[0] % 4)
                    psrot[0] += 1
                    for k in range(KT):
                        P.op("pe", [("wb", ws), "hT"], [("ps", pi)],
                             lambda e: e.matmul(ps[pi].a(0, [[1, n]]),
                                                lhsT=wb.a(wo_ + k * 512 + ti * 128, [[1, 128]]),
                                                rhs=hT.a(k * TT + c0, [[1, n]]), start=(k == 0), stop=(k == KT - 1)))
                    t0 = c0 - HALO
                    if ct < 2:
                        P.op("act", [("ps", pi)], [("ut", ct)],
                             lambda e: e.activation(out=ut.a(ct * TT + c0, [[1, n]]), in_=ps[pi].a(0, [[1, n]]),
                                                    func=AF.Copy))
                    elif ct < 6:
                        P.op("act", [("ps", pi)], [("zs", ct - 2)],
                             lambda e: e.activation(out=zs.a((ct - 2) * T + t0, [[1, n]]), in_=ps[pi].a(0, [[1, n]]),
                                                    func=AF.Silu))
                    elif ct < 18:
                        P.op("act", [("ps", pi)], [("cpre", cs_)],
                             lambda e: e.activation(out=cpre.a(c0, [[1, n]]), in_=ps[pi].a(0, [[1, n]]), func=AF.Copy))
                    else:
                        P.op("act", [("ps", pi)], [("so", ct - 18)],
                             lambda e: e.activation(out=so.a((ct - 18) * T + t0, [[1, n]]), in_=ps[pi].a(0, [[1, n]]),
                                                    func=AF.Sigmoid))
                if is_conv:
                    if ct < 14:
                        wcol = PP_CXBC + (ct - 6) * 5
                        if ct < 10:
                            dest, dkey = xs.a((ct - 6) * T, [[1, T]]), ("xs", ct - 6)
                        else:
                            dest, dkey = BC.a((ct - 10) * T, [[1, T]]), ("BC", ct - 10)
                    else:
                        wcol = PP_CQK + (ct - 14) * 5
                        dest, dkey = qk.a((ct - 14) * T, [[1, T]]), ("qk", ct - 14)
                    P.op("act", [("cpre", cs_), ("ppt", l)], [("ctmp", cs_)],
                         lambda e: e.activation(out=ctmp.a(cs_ * T, [[1, T]]), in_=cpre.a(HALO, [[1, T]]), func=AF.Identity,
                                                bias=pp(l, wcol + 4), scale=pp(l, wcol + 3)))
                    for tap in range(3):
                        P.op("dve", [("cpre", cs_), ("ctmp", cs_), ("ppt", l)], [("ctmp", cs_)],
                             lambda e: e.scalar_tensor_tensor(out=ctmp.a(cs_ * T, [[1, T]]), in0=cpre.a(HALO - 3 + tap, [[1, T]]),
                                                              scalar=pp(l, wcol + tap), in1=ctmp.a(cs_ * T, [[1, T]]),
                                                              op0=ALU.mult, op1=ALU.add))
                    P.op("act", [("ctmp", cs_)], [dkey], lambda e: e.activation(out=dest, in_=ctmp.a(cs_ * T, [[1, T]]), func=AF.Silu))
        for c in range(0 if dbg_skip_p1 else NCH):
            pi = 2 + (psrot[0] % 4)
            psrot[0] += 1
            for k in range(KT):
                P.op("pe", ["wvgb", "hT"], [("ps", pi)],
                     lambda e: e.matmul(ps[pi].a(0, [[1, 272]]), lhsT=hT.a(k * TT + HALO + c * 128, [[1, 128]]),
                                        rhs=wvgb.a(k * 272, [[1, 272]]), start=(k == 0), stop=(k == KT - 1)))
            P.op("act", [("ps", pi)], ["vtok"],
                 lambda e: e.activation(out=vtok.a(c * 256, [[1, 256]]), in_=ps[pi].a(0, [[1, 256]]), func=AF.Copy))
            P.op("dve", [("ps", pi)], ["graw"],
                 lambda e: e.tensor_copy(out=graw.a(c * 16, [[1, 16]]), in_=ps[pi].a(256, [[1, 16]])))
        P.barrier()
        if l == 0:
            dbg_dump("zs", zs, BF)
            dbg_dump("xs", xs, BF)
            dbg_dump("BC", BC, BF)
            dbg_dump("qk", qk, BF)
            dbg_dump("so", so, BF)
            dbg_dump("vtok", vtok, BF)
            dbg_dump("graw", graw, F32)
            dbg_dump("ut", ut, F32)
        P.free([t3, t4, t5, t6, t7])
        if stop_after == "P1":
            break

        pA, q1 = P.sb("pA", [128, 2, TT], F32)
        pB, q2 = P.sb("pB", [128, 2, TT], F32)
        pooled, q3 = P.sb("pooled", [128, 2, T], BF)
        p16, q4 = P.sb("p16", [128, 2, 16], F32)
        UK = [("ut", 0), ("ut", 1)]
        P.op("dve", UK, ["pA"], lambda e: e.tensor_tensor(out=pA.a(1, [[TT, 2], [1, TT - 1]]), in0=ut.a(1, [[TT, 2], [1, TT - 1]]),
                                                         in1=ut.a(0, [[TT, 2], [1, TT - 1]]), op=ALU.add))
        P.op("dve", ["pA"], ["pB"], lambda e: e.tensor_tensor(out=pB.a(3, [[TT, 2], [1, TT - 3]]), in0=pA.a(3, [[TT, 2], [1, TT - 3]]),
                                                           in1=pA.a(1, [[TT, 2], [1, TT - 3]]), op=ALU.add))
        P.op("dve", ["pB"], ["pA"], lambda e: e.tensor_tensor(out=pA.a(TT + 7, [[1, TT - 7]]), in0=pB.a(TT + 7, [[1, TT - 7]]),
                                                           in1=pB.a(TT + 3, [[1, TT - 7]]), op=ALU.add))
        P.op("dve", ["pA"], ["pB"], lambda e: e.tensor_tensor(out=pB.a(TT + 15, [[1, TT - 15]]), in0=pA.a(TT + 15, [[1, TT - 15]]),
                                                           in1=pA.a(TT + 7, [[1, TT - 15]]), op=ALU.add))
        for i in range(2):
            for h in range(2):
                srct = pA if h == 0 else pB
                hp = dict(p0=h * 64, pn=64)
                P.op("dve", ["pA", "pB", ("ppt", l)] + UK, ["pooled"],
                     lambda e: e.scalar_tensor_tensor(out=pooled.a(i * T, [[1, T]], **hp), in0=srct.a(i * TT + HALO, [[1, T]], **hp),
                                                      scalar=ppt.a(l * NPP + PP_INVW + i, [[1, 1]], **hp),
                                                      in1=ut.a(i * TT + HALO, [[1, T]], **hp), op0=ALU.mult, op1=ALU.subtract))
                P.op("dve", ["pA", "pB", "cmt"], ["p16"],
                     lambda e: e.tensor_tensor(out=p16.a(i * 16, [[1, 16]], **hp), in0=srct.a(i * TT + HALO, [[1, 16]], **hp),
                                               in1=cmt.a(CM_INVC + i * 16, [[1, 16]], **hp), op=ALU.mult))
                P.op("dve", ["p16"] + UK, ["pooled"],
                     lambda e: e.tensor_tensor(out=pooled.a(i * T, [[1, 16]], **hp), in0=p16.a(i * 16, [[1, 16]], **hp),
                                               in1=ut.a(i * TT + HALO, [[1, 16]], **hp), op=ALU.subtract))
        for i in range(2):
            for bk in range(4):
                pi = 2 + (psrot[0] % 4)
                psrot[0] += 1
                P.op("pe", ["pooled", "pwb"], [("ps", pi)],
                     lambda e: e.matmul(ps[pi].a(), lhsT=pwb.a(i * 128, [[1, 128]]), rhs=pooled.a(i * T + bk * 512, [[1, 512]]),
                                        start=True, stop=True))
                P.op("dve", [("ps", pi), ("ppt", l)], [("ypool", i)],
                     lambda e: e.tensor_scalar(out=ypool.a(i * T + bk * 512, [[1, 512]]), in0=ps[pi].a(),
                                               scalar1=pp(l, PP_POOLB + i), scalar2=pp(l, PP_POOLS + i),
                                               op0=ALU.add, op1=ALU.mult))
        P.barrier()
        if l == 0:
            dbg_dump("ypool", ypool, BF)
        P.free([t1, t8, t2, q1, q2, q3, q4])
        if stop_after == "P2":
            break

        gl = []

        def G(name, w, dt=F32):
            tl, cmx = P.sb(name, [128, w], dt)
            gl.append(cmx)
            return tl
        agbuf = G("agbuf", AGW)
        last16 = G("last16", 128)
        gat = G("gat", 192)
        gat_hi = G("gat_hi", 192, BF)
        gat_lo = G("gat_lo", 192, BF)
        li_all = G("li_all", 64)
        cs_all = G("cs_all", 192)
        tot_all = G("tot_all", 192)
        dt_all = G("dt_all", 128)
        dte = G("dte", 128)
        wloc = G("wloc", 64)
        lib = G("lib", 64)
        Etot = G("Etot", 128)
        Etf = G("Etf", 64)
        EtotM = G("EtotM", 32)
        cum = G("cum", 192)
        Ecum = G("Ecum", 128)
        Ecf = G("Ecf", 64)
        EcumM = G("EcumM", 32)
        tmpg = G("tmpg", 192)
        eA = G("eA", 8)
        ldf = G("ldf", 4)
        PB = ("pbt", l)
        P.op("dve", [], ["agbuf"], lambda e: e.memset(agbuf.a(), 0.0))
        P.op("dve", [], ["cum"], lambda e: e.memset(cum.a(), 0.0))
        P.op("dve", ["graw", PB], ["tmpg"], lambda e: e.tensor_tensor(out=tmpg.a(0, [[8, 16], [1, 8]]), in0=graw.a(0, [[16, 16], [1, 8]]),
                                                                   in1=pbt.a(l * NPB + 0, [[0, 16], [1, 8]]), op=ALU.add))
        P.op("act", ["tmpg"], ["tmpg"], lambda e: e.activation(out=tmpg.a(0, [[1, 128]]), in_=tmpg.a(0, [[1, 128]]), func=AF.Exp))
        P.op("act", ["tmpg", "epst"], ["dt_all"], lambda e: e.activation(out=dt_all.a(), in_=tmpg.a(0, [[1, 128]]), func=AF.Ln, bias=one_ap()))
        P.op("act", [PB], ["eA"], lambda e: e.activation(out=eA.a(), in_=pbt.a(l * NPB + 8, [[1, 8]]), func=AF.Exp))
        P.op("dve", ["dt_all", "eA"], ["gat"], lambda e: e.scalar_tensor_tensor(out=gat.a(0, [[8, 16], [1, 8]]), in0=dt_all.a(0, [[8, 16], [1, 8]]),
                                                                             scalar=-1.0, in1=eA.a(0, [[0, 16], [1, 8]]), op0=ALU.mult, op1=ALU.mult))
        P.op("dve", ["graw", PB], ["tmpg"], lambda e: e.tensor_tensor(out=tmpg.a(128, [[4, 16], [1, 4]]), in0=graw.a(12, [[16, 16], [1, 4]]),
                                                                   in1=pbt.a(l * NPB + 20, [[0, 16], [1, 4]]), op=ALU.add))
        P.op("act", ["tmpg"], ["tmpg"], lambda e: e.activation(out=tmpg.a(128, [[1, 64]]), in_=tmpg.a(128, [[1, 64]]), func=AF.Exp, scale=-1.0))
        P.op("act", ["tmpg", "epst"], ["tmpg"], lambda e: e.activation(out=tmpg.a(128, [[1, 64]]), in_=tmpg.a(128, [[1, 64]]), func=AF.Ln, bias=one_ap()))
        P.op("dve", ["tmpg"], ["gat"], lambda e: e.tensor_scalar(out=gat.a(128, [[1, 64]]), in0=tmpg.a(128, [[1, 64]]), scalar1=-1.0, scalar2=None, op0=ALU.mult))
        P.op("dve", ["graw", PB], ["li_all"], lambda e: e.tensor_tensor(out=li_all.a(0, [[4, 16], [1, 4]]), in0=graw.a(8, [[16, 16], [1, 4]]),
                                                                     in1=pbt.a(l * NPB + 16, [[0, 16], [1, 4]]), op=ALU.add))
        P.op("dve", ["gat"], ["gat_hi"], lambda e: e.tensor_copy(out=gat_hi.a(), in_=gat.a()))
        P.op("dve", ["gat", "gat_hi"], ["gat_lo"], lambda e: e.tensor_tensor(out=gat_lo.a(), in0=gat.a(), in1=gat_hi.a(), op=ALU.subtract))
        P.op("pe", ["gat_hi", "U_bf"], [("ps", 0)], lambda e: e.matmul(ps[0].a(0, [[1, 192]]), lhsT=U_bf.a(), rhs=gat_hi.a(), start=True, stop=False))
        P.op("pe", ["gat_lo", "U_bf"], [("ps", 0)], lambda e: e.matmul(ps[0].a(0, [[1, 192]]), lhsT=U_bf.a(), rhs=gat_lo.a(), start=False, stop=True))
        P.op("pe", ["gat_hi", "ones_bf"], [("ps", 1)], lambda e: e.matmul(ps[1].a(0, [[1, 192]]), lhsT=ones_bf.a(), rhs=gat_hi.a(), start=True, stop=False))
        P.op("pe", ["gat_lo", "ones_bf"], [("ps", 1)], lambda e: e.matmul(ps[1].a(0, [[1, 192]]), lhsT=ones_bf.a(), rhs=gat_lo.a(), start=False, stop=True))
        P.op("act", [("ps", 0)], ["cs_all"], lambda e: e.activation(out=cs_all.a(), in_=ps[0].a(0, [[1, 192]]), func=AF.Copy))
        P.op("act", [("ps", 1)], ["tot_all"], lambda e: e.activation(out=tot_all.a(), in_=ps[1].a(0, [[1, 192]]), func=AF.Copy))
        P.op("dve", ["tot_all", "cs_all"], ["tmpg"], lambda e: e.tensor_tensor(out=tmpg.a(0, [[1, 128]]), in0=tot_all.a(0, [[1, 128]]), in1=cs_all.a(0, [[1, 128]]), op=ALU.subtract))
        P.op("act", ["tmpg"], ["dte"], lambda e: e.activation(out=dte.a(), in_=tmpg.a(0, [[1, 128]]), func=AF.Exp))
        P.op("dve", ["li_all", "cs_all"], ["lib"], lambda e: e.tensor_tensor(out=lib.a(), in0=li_all.a(), in1=cs_all.a(128, [[1, 64]]), op=ALU.subtract))
        P.op("dve", ["lib", "tot_all"], ["tmpg"], lambda e: e.tensor_tensor(out=tmpg.a(128, [[1, 64]]), in0=lib.a(), in1=tot_all.a(128, [[1, 64]]), op=ALU.add))
        P.op("act", ["tmpg"], ["wloc"], lambda e: e.activation(out=wloc.a(), in_=tmpg.a(128, [[1, 64]]), func=AF.Exp))
        P.op("act", ["tot_all"], ["Etot"], lambda e: e.activation(out=Etot.a(), in_=tot_all.a(0, [[1, 128]]), func=AF.Exp))
        P.op("act", ["tot_all"], ["Etf"], lambda e: e.activation(out=Etf.a(), in_=tot_all.a(128, [[1, 64]]), func=AF.Exp))
        for hp_ in range(2):
            P.op("dve", ["Etf"], ["EtotM"], lambda e: e.tensor_copy(out=EtotM.a(0, [[2, 16], [1, 2]], p0=hp_ * 64, pn=64),
                                                                   in_=Etf.a(hp_, [[4, 16], [2, 2]], p0=hp_ * 64, pn=64)))
        for (o0, w) in ((0, 8), (128, 4)):
            srcs = [tot_all, tmpg, cum, tmpg, cum]
            for step in range(4):
                sh = (1 << step)
                a_, b_ = srcs[step], srcs[step + 1]
                P.op("dve", ["tot_all", "tmpg", "cum"], ["tmpg", "cum"], lambda e: e.tensor_copy(out=b_.a(o0, [[1, sh * w]]), in_=a_.a(o0, [[1, sh * w]])))
                P.op("dve", ["tot_all", "tmpg", "cum"], ["tmpg", "cum"], lambda e: e.tensor_tensor(out=b_.a(o0 + sh * w, [[1, (NCH - sh) * w]]), in0=a_.a(o0 + sh * w, [[1, (NCH - sh) * w]]),
                                                                                                in1=a_.a(o0, [[1, (NCH - sh) * w]]), op=ALU.add))
            P.op("dve", ["cum", "tot_all"], ["cum"], lambda e: e.tensor_tensor(out=cum.a(o0, [[1, NCH * w]]), in0=cum.a(o0, [[1, NCH * w]]), in1=tot_all.a(o0, [[1, NCH * w]]), op=ALU.subtract))
        P.op("act", ["cum"], ["Ecum"], lambda e: e.activation(out=Ecum.a(), in_=cum.a(0, [[1, 128]]), func=AF.Exp))
        P.op("act", ["cum"], ["Ecf"], lambda e: e.activation(out=Ecf.a(), in_=cum.a(128, [[1, 64]]), func=AF.Exp))
        for hp_ in range(2):
            P.op("dve", ["Ecf"], ["EcumM"], lambda e: e.tensor_copy(out=EcumM.a(0, [[2, 16], [1, 2]], p0=hp_ * 64, pn=64),
                                                                   in_=Ecf.a(hp_, [[4, 16], [2, 2]], p0=hp_ * 64, pn=64)))
        P.op("dve", ["cum", "tot_all", "agbuf"], ["agbuf"], lambda e: e.tensor_tensor(out=agbuf.a(512, [[1, 8]]), in0=cum.a(15 * 8, [[1, 8]]),
                                                                                   in1=tot_all.a(15 * 8, [[1, 8]]), op=ALU.add))
        P.op("dve", ["cum", "tot_all"], ["ldf"], lambda e: e.tensor_tensor(out=ldf.a(), in0=cum.a(128 + 15 * 4, [[1, 4]]),
                                                                        in1=tot_all.a(128 + 15 * 4, [[1, 4]]), op=ALU.add))
        for hp_ in range(2):
            P.op("dve", ["ldf", "agbuf"], ["agbuf"], lambda e: e.tensor_copy(out=agbuf.a(776, [[1, 2]], p0=hp_ * 64, pn=64),
                                                                            in_=ldf.a(hp_, [[2, 2]], p0=hp_ * 64, pn=64)))
        P.barrier()
        if stop_after == "P3":
            break

        Xdt, a1 = P.sb("Xdt", [128, NCH, 512], BF)
        Pc, a2 = P.sb("Pc", [128, NCH, 512], BF)
        PMc, a3 = P.sb("PMc", [128, NCH, 256], BF)
        Xdtp, a4 = P.sb("Xdtp", [128, 2, 512], BF)
        Btok, a5 = P.sb("Btok", [128, 2, 256], BF)
        kp, a6 = P.sb("kp", [128, 2, 256], BF)
        for c in range(dbg_nch):
            s = c % 2
            ch0 = c * 128
            for i in range(4):
                P.op("pe", [("xs", i), "ident_bf"], ["psT"], lambda e: e.matmul(ps[2].a(i * 128, [[1, 128]]), lhsT=xs.a(i * T + ch0, [[1, 128]]), rhs=ident_bf.a(), start=True, stop=True))
            for g in range(2):
                P.op("pe", [("BC", g), "ident_bf"], ["psT"], lambda e: e.matmul(ps[3].a(g * 128, [[1, 128]]), lhsT=BC.a(g * T + ch0, [[1, 128]]), rhs=ident_bf.a(), start=True, stop=True))
            for j in range(2):
                P.op("pe", [("qk", 2 + j), "ident_bf"], ["psT"], lambda e: e.matmul(ps[3].a(256 + j * 128, [[1, 128]]), lhsT=qk.a((2 + j) * T + ch0, [[1, 128]]), rhs=ident_bf.a(), start=True, stop=True))
            P.op("dve", ["psT", "dt_all"], [("Xdt", c)], lambda e: e.tensor_tensor(out=Xdt.a(c * 512, [[64, 8], [1, 64]]), in0=ps[2].a(0, [[64, 8], [1, 64]]),
                                                                              in1=dt_all.a(c * 8, [[1, 8], [0, 64]]), op=ALU.mult))
            P.op("dve", [("Xdt", c), "dte"], [("Xdtp", s)], lambda e: e.tensor_tensor(out=Xdtp.a(s * 512, [[64, 8], [1, 64]]), in0=Xdt.a(c * 512, [[64, 8], [1, 64]]),
                                                                                  in1=dte.a(c * 8, [[1, 8], [0, 64]]), op=ALU.mult))
            P.op("act", ["psT"], [("Btok", s)], lambda e: e.activation(out=Btok.a(s * 256, [[1, 256]]), in_=ps[3].a(0, [[1, 256]]), func=AF.Copy))
            P.op("dve", ["psT", "wloc"], [("kp", s)], lambda e: e.tensor_tensor(out=kp.a(s * 256, [[64, 4], [1, 64]]), in0=ps[3].a(256, [[64, 4], [1, 64]]),
                                                                           in1=wloc.a(c * 4, [[1, 4], [0, 64]]), op=ALU.mult))
            for g in range(2):
                P.op("pe", [("Btok", s), ("Xdtp", s)], [("ps", 0)], lambda e: e.matmul(ps[0].a(g * 256, [[1, 256]]), lhsT=Btok.a(s * 256 + g * 128, [[1, 128]]),
                                                                                   rhs=Xdtp.a(s * 512 + g * 256, [[1, 256]]), start=True, stop=True))
            for h in range(4):
                j, hp_ = h // 2, h % 2
                P.op("pe", [("kp", s), "vtok"], [("ps", 1)], lambda e: e.matmul(ps[1].a(j * 128, [[1, 64]], p0=hp_ * 64, pn=64), lhsT=kp.a(s * 256 + h * 64, [[1, 64]]),
                                                                            rhs=vtok.a(c * 256 + h * 64, [[1, 64]]), start=True, stop=True))
                P.op("pe", [("kp", s), "ones_bf"], [("ps", 1)], lambda e: e.matmul(ps[1].a(j * 128 + 64, [[1, 64]], p0=hp_ * 64, pn=64), lhsT=kp.a(s * 256 + h * 64, [[1, 64]]),
                                                                               rhs=ones_bf.a(0, [[1, 64]]), start=True, stop=True))
            P.op("act", ["agbuf"], [("Pc", c)], lambda e: e.activation(out=Pc.a(c * 512, [[1, 512]]), in_=agbuf.a(0, [[1, 512]]), func=AF.Copy))
            P.op("act", ["agbuf"], [("PMc", c)], lambda e: e.activation(out=PMc.a(c * 256, [[1, 256]]), in_=agbuf.a(520, [[1, 256]]), func=AF.Copy))
            P.op("dve", ["agbuf", "Etot"], ["agbuf"], lambda e: e.tensor_tensor(out=agbuf.a(0, [[64, 8], [1, 64]]), in0=agbuf.a(0, [[64, 8], [1, 64]]),
                                                                             in1=Etot.a(c * 8, [[1, 8], [0, 64]]), op=ALU.mult))
            P.op("dve", ["agbuf", ("ps", 0)], ["agbuf"], lambda e: e.tensor_tensor(out=agbuf.a(0, [[1, 512]]), in0=agbuf.a(0, [[1, 512]]), in1=ps[0].a(), op=ALU.add))
            P.op("dve", ["agbuf", "EtotM"], ["agbuf"], lambda e: e.tensor_tensor(out=agbuf.a(520, [[128, 2], [1, 128]]), in0=agbuf.a(520, [[128, 2], [1, 128]]),
                                                                              in1=EtotM.a(c * 2, [[1, 2], [0, 128]]), op=ALU.mult))
            P.op("dve", ["agbuf", ("ps", 1)], ["agbuf"], lambda e: e.tensor_tensor(out=agbuf.a(520, [[1, 256]]), in0=agbuf.a(520, [[1, 256]]), in1=ps[1].a(0, [[1, 256]]), op=ALU.add))
        P.barrier()
        P.free([a4, a5, a6])
        if stop_after == "PA":
            break

        Sin, b2 = P.sb("Sin", [128, 512], F32)
        SinM, b3 = P.sb("SinM", [128, 256], F32)
        agin, b1 = P.sb("agin", [128, 2, AGW], F32)
        coef, b4 = P.sb("coef", [128, 32], F32)
        P.op("dve", [], ["Sin"], lambda e: e.memset(Sin.a(), 0.0))
        P.op("dve", [], ["SinM"], lambda e: e.memset(SinM.a(), 0.0))
        P.dma("pool", cc_in[agb].a(), agbuf.a(), ["agbuf"], [("ccin", agb)])
        P.allgather(cc_in[agb], cc_out[agb], [("ccin", agb)], [("ccout", agb)], ("ag", agb))
        for r in range(NCORES):
            s = r % 2
            P.dma("pool", agin.a(s * AGW, [[1, AGW]]), cc_out[agb].a(0, [[1, AGW]], p0=r * 128, pn=128), [("ccout", agb)], [("agin", s)])
            P.op("act", [("agin", s)], ["coef"], lambda e: e.activation(out=coef.a(0, [[1, 8]]), in_=agin.a(s * AGW + 512, [[1, 8]]), func=AF.Exp))
            P.op("act", [("agin", s), "coef"], ["coef"], lambda e: e.activation(out=coef.a(8, [[1, 2]]), in_=agin.a(s * AGW + 776, [[1, 2]]), func=AF.Exp))
            P.op("dve", ["coef", "cmt"], ["coef"], lambda e: e.tensor_scalar(out=coef.a(16, [[1, 10]]), in0=coef.a(0, [[1, 10]]), scalar1=cmc(CM_PREC + r),
                                                                            scalar2=cmc(CM_NPREC + r), op0=ALU.mult, op1=ALU.add))
            P.op("dve", ["coef", "Sin"], ["Sin"], lambda e: e.tensor_tensor(out=Sin.a(0, [[64, 8], [1, 64]]), in0=Sin.a(0, [[64, 8], [1, 64]]),
                                                                         in1=coef.a(16, [[1, 8], [0, 64]]), op=ALU.mult))
            P.op("dve", [("agin", s), "cmt", "Sin"], ["Sin"], lambda e: e.scalar_tensor_tensor(out=Sin.a(), in0=agin.a(s * AGW, [[1, 512]]), scalar=cmc(CM_PREC + r),
                                                                                            in1=Sin.a(), op0=ALU.mult, op1=ALU.add))
            P.op("dve", ["coef", "SinM"], ["SinM"], lambda e: e.tensor_tensor(out=SinM.a(0, [[128, 2], [1, 128]]), in0=SinM.a(0, [[128, 2], [1, 128]]),
                                                                           in1=coef.a(24, [[1, 2], [0, 128]]), op=ALU.mult))
            P.op("dve", [("agin", s), "cmt", "SinM"], ["SinM"], lambda e: e.scalar_tensor_tensor(out=SinM.a(), in0=agin.a(s * AGW + 520, [[1, 256]]), scalar=cmc(CM_PREC + r),
                                                                                              in1=SinM.a(), op0=ALU.mult, op1=ALU.add))
        P.barrier()
        P.free([b1, b4])
        if stop_after == "AG1":
            break
        if l == 0:
            dbg_dump("Sin", Sin, F32)
            dbg_dump("SinM", SinM, F32)

        def S2(name, shape, dt):
            tl, cmx = P.sb(name, shape, dt)
            bl.append(cmx)
            return tl
        bl = []
        tmpS = S2("tmpS", [128, 512], F32)
        prevb = S2("prevb", [128, 2, 512], BF)
        aU = S2("aU", [128, 2, 1024], BF)
        M2 = S2("M2", [128, 1, 1024], F32)
        LT = S2("LT", [128, 2, 1024], BF)
        E1 = S2("E1", [128, 2, 1024], BF)
        Cp = S2("Cp", [128, 2, 1024], BF)
        MT = S2("MT", [128, 2, 1024], BF)
        STs = S2("STs", [128, 256], F32)
        tmpY = S2("tmpY", [128, 512], F32)
        ygrp = S2("ygrp", [128, 4, 256], F32)
        sqg = S2("sqg", [128, 4, 256], BF)
        lng = S2("lng", [128, 256], F32)
        rsg = S2("rsg", [128, 256], F32)
        prevMb = S2("prevMb", [128, 2, 256], BF)
        fU = S2("fU", [128, 2, 512], BF)
        M2m = S2("M2m", [128, 1, 512], F32)
        WT = S2("WT", [128, 2, 512], BF)
        E1m = S2("E1m", [128, 2, 512], BF)
        qp = S2("qp", [128, 2, 512], BF)
        qz = S2("qz", [128, 2, 512], BF)
        P.op("dve", [], [("qp", 0), ("qp", 1)], lambda e: e.memset(qp.a(), 0.0))
        P.op("dve", [], [("qz", 0), ("qz", 1)], lambda e: e.memset(qz.a(), 0.0))
        PT = S2("PT", [128, 2, 512], BF)
        dent = S2("dent", [128, 256], F32)
        hgrp = S2("hgrp", [128, 2, 256], F32)
        XSK = [("xs", i) for i in range(4)]
        ZSK = [("zs", i) for i in range(4)]
        for c in range(dbg_nch):
            s = c % 2
            cc = c % 2
            ch0 = c * 128
            g0 = (c - 1) * 128
            P.op("dve", ["Sin", "Ecum"], ["tmpS"], lambda e: e.tensor_tensor(out=tmpS.a(0, [[64, 8], [1, 64]]), in0=Sin.a(0, [[64, 8], [1, 64]]),
                                                                          in1=Ecum.a(c * 8, [[1, 8], [0, 64]]), op=ALU.mult))
            P.op("dve", ["tmpS", ("Pc", c)], [("prevb", s)], lambda e: e.tensor_tensor(out=prevb.a(s * 512, [[1, 512]]), in0=tmpS.a(), in1=Pc.a(c * 512, [[1, 512]]), op=ALU.add))
            for hl, gsrc in enumerate((gat_hi, gat_lo)):
                P.op("dve", ["U_bf", "gat_hi", "gat_lo"], [("aU", hl)], lambda e: e.tensor_tensor(out=aU.a(hl * 1024, [[128, 8], [1, 128]]), in0=U_bf.a(0, [[0, 8], [1, 128]]),
                                                                                              in1=gsrc.a(c * 8, [[1, 8], [0, 128]]), op=ALU.mult))
            for hf in range(2):
                for hl in range(2):
                    P.op("pe", [("aU", hl), "ones_bf"], [("ps", hf)], lambda e: e.matmul(ps[hf].a(), lhsT=ones_bf.a(), rhs=aU.a(hl * 1024 + hf * 512, [[1, 512]]),
                                                                                     start=(hl == 0), stop=(hl == 1)))
            P.op("dve", ["consts", "cs_all"], ["M2"], lambda e: e.tensor_tensor(out=M2.a(0 * 1024, [[128, 8], [1, 128]]), in0=consts.a(C_MASK, [[0, 8], [1, 128]]),
                                                                                  in1=cs_all.a(c * 8, [[1, 8], [0, 128]]), op=ALU.subtract))
            for hf in range(2):
                P.op("dve", [("ps", hf), "M2"], ["M2"], lambda e: e.tensor_tensor(out=M2.a(hf * 512, [[1, 512]]), in0=ps[hf].a(), in1=M2.a(0 * 1024 + hf * 512, [[1, 512]]), op=ALU.add))
            P.op("act", ["M2"], [("LT", s)], lambda e: e.activation(out=LT.a(s * 1024, [[1, 1024]]), in_=M2.a(), func=AF.Exp))
            for hf in range(2):
                P.op("act", [("ps", hf)], [("E1", s)], lambda e: e.activation(out=E1.a(s * 1024 + hf * 512, [[1, 512]]), in_=ps[hf].a(), func=AF.Exp))
            for g in range(2):
                P.op("dve", [("BC", 2 + g), ("E1", s)], [("Cp", s)], lambda e: e.tensor_tensor(out=Cp.a(s * 1024 + g * 512, [[128, 4], [1, 128]]), in0=BC.a((2 + g) * T + ch0, [[0, 4], [1, 128]]),
                                                                                          in1=E1.a(s * 1024 + g * 512, [[128, 4], [1, 128]]), op=ALU.mult))
            for g in range(2):
                P.op("pe", [("BC", g), ("BC", 2 + g)], [("ps", 2, "a")], lambda e: e.matmul(ps[2].a(g * 128, [[1, 128]]), lhsT=BC.a(g * T + ch0, [[1, 128]]),
                                                                                       rhs=BC.a((2 + g) * T + ch0, [[1, 128]]), start=True, stop=True))
            P.op("act", [("ps", 2, "a")], ["STs"], lambda e: e.activation(out=STs.a(), in_=ps[2].a(0, [[1, 256]]), func=AF.Copy))
            for g in range(2):
                P.op("dve", ["STs", ("LT", s)], [("MT", s)], lambda e: e.tensor_tensor(out=MT.a(s * 1024 + g * 512, [[128, 4], [1, 128]]), in0=STs.a(g * 128, [[0, 4], [1, 128]]),
                                                                                  in1=LT.a(s * 1024 + g * 512, [[128, 4], [1, 128]]), op=ALU.mult))
            for h in range(8):
                i, hp_ = h // 2, h % 2
                P.op("pe", [("Xdt", c), ("MT", s)], [("ps", 3)], lambda e: e.matmul(ps[3].a(i * 128, [[1, 128]], p0=hp_ * 64, pn=64), lhsT=Xdt.a(c * 512 + h * 64, [[1, 64]]),
                                                                                rhs=MT.a(s * 1024 + h * 128, [[1, 128]]), start=True, stop=False))
                P.op("pe", [("prevb", s), ("Cp", s)], [("ps", 3)], lambda e: e.matmul(ps[3].a(i * 128, [[1, 128]], p0=hp_ * 64, pn=64), lhsT=prevb.a(s * 512 + h * 64, [[1, 64]]),
                                                                                  rhs=Cp.a(s * 1024 + h * 128, [[1, 128]]), start=False, stop=True))
            P.op("dve", XSK + [("ppt", l)], ["tmpY"], lambda e: e.tensor_tensor(out=tmpY.a(0, [[128, 4], [1, 128]]), in0=xs.a(ch0, [[T, 4], [1, 128]]),
                                                                             in1=ppt.a(l * NPP + PP_SSDD, [[1, 4], [0, 128]]), op=ALU.mult))
            P.op("dve", ["tmpY", ("ps", 3)], ["ygrp"], lambda e: e.tensor_tensor(out=ygrp.a(cc * 128, [[256, 4], [1, 128]]), in0=ps[3].a(0, [[128, 4], [1, 128]]),
                                                                              in1=tmpY.a(0, [[128, 4], [1, 128]]), op=ALU.add))
            if cc == 1:
                P.op("dve", ["ygrp"] + ZSK, ["ygrp"], lambda e: e.tensor_tensor(out=ygrp.a(0, [[256, 4], [1, 256]]), in0=ygrp.a(0, [[256, 4], [1, 256]]),
                                                                             in1=zs.a(g0, [[T, 4], [1, 256]]), op=ALU.mult))
                P.op("act", ["ygrp"], ["sqg"], lambda e: e.activation(out=sqg.a(), in_=ygrp.a(), func=AF.Square))
                for i in range(4):
                    P.op("pe", ["sqg", "ones_bf"], [("ps", 2, "b")], lambda e: e.matmul(ps[2].a(256, [[1, 256]]), lhsT=ones_bf.a(), rhs=sqg.a(i * 256, [[1, 256]]),
                                                                                    start=(i == 0), stop=(i == 3)))
                rms_rstd(ps[2].a(256, [[1, 256]]), 512.0, rsg.a(), lng.a(), ("ps", 2, "b"), "rsg", "lng")
                for i in range(4):
                    P.op("dve", ["ygrp", "rsg", ("ppt", l)], [("xs", i)], lambda e: e.scalar_tensor_tensor(out=xs.a(i * T + g0, [[1, 256]]), in0=ygrp.a(i * 256, [[1, 256]]),
                                                                                                        scalar=pp(l, PP_SSDN + i), in1=rsg.a(), op0=ALU.mult, op1=ALU.mult))
            P.op("dve", ["SinM", "EcumM"], ["tmpS"], lambda e: e.tensor_tensor(out=tmpS.a(0, [[128, 2], [1, 128]]), in0=SinM.a(0, [[128, 2], [1, 128]]),
                                                                            in1=EcumM.a(c * 2, [[1, 2], [0, 128]]), op=ALU.mult))
            P.op("dve", ["tmpS", ("PMc", c)], [("prevMb", s)], lambda e: e.tensor_tensor(out=prevMb.a(s * 256, [[1, 256]]), in0=tmpS.a(0, [[1, 256]]), in1=PMc.a(c * 256, [[1, 256]]), op=ALU.add))
            for hl, gsrc in enumerate((gat_hi, gat_lo)):
                P.op("dve", ["U_bf", "gat_hi", "gat_lo"], [("fU", hl)], lambda e: e.tensor_tensor(out=fU.a(hl * 512, [[128, 4], [1, 128]]), in0=U_bf.a(0, [[0, 4], [1, 128]]),
                                                                                              in1=gsrc.a(128 + c * 4, [[1, 4], [0, 128]]), op=ALU.mult))
            for hl in range(2):
                P.op("pe", [("fU", hl), "ones_bf"], [("ps", 4)], lambda e: e.matmul(ps[4].a(), lhsT=ones_bf.a(), rhs=fU.a(hl * 512, [[1, 512]]), start=(hl == 0), stop=(hl == 1)))
            P.op("dve", ["consts", "lib"], ["M2m"], lambda e: e.tensor_tensor(out=M2m.a(0 * 512, [[128, 4], [1, 128]]), in0=consts.a(C_MASK, [[0, 4], [1, 128]]),
                                                                                in1=lib.a(c * 4, [[1, 4], [0, 128]]), op=ALU.add))
            P.op("dve", [("ps", 4), "M2m"], ["M2m"], lambda e: e.tensor_tensor(out=M2m.a(), in0=ps[4].a(), in1=M2m.a(0 * 512, [[1, 512]]), op=ALU.add))
            P.op("act", ["M2m", "epst"], [("WT", s)], lambda e: e.activation(out=WT.a(s * 512, [[1, 512]]), in_=M2m.a(), func=AF.Exp, bias=ln8_ap()))
            P.op("act", [("ps", 4), "epst"], [("E1m", s)], lambda e: e.activation(out=E1m.a(s * 512, [[1, 512]]), in_=ps[4].a(), func=AF.Exp, bias=ln8_ap()))
            for hp_ in range(2):
                hpd = dict(p0=hp_ * 64, pn=64)
                P.op("dve", [("qk", 0), ("qk", 1), ("E1m", s)], [("qp", s)], lambda e: e.tensor_tensor(out=qp.a(s * 512 + hp_ * 128, [[256, 2], [1, 128]], **hpd), in0=qk.a(ch0, [[T, 2], [1, 128]], **hpd),
                                                                                                    in1=E1m.a(s * 512 + hp_ * 128, [[256, 2], [1, 128]], **hpd), op=ALU.mult))
                P.op("act", [("qk", 0), ("qk", 1)], [("qz", s)], lambda e: e.activation(out=qz.a(s * 512 + hp_ * 128, [[256, 2], [1, 128]], **hpd), in_=qk.a(ch0, [[T, 2], [1, 128]], **hpd), func=AF.Copy))
            for h in range(4):
                j, hp_ = h // 2, h % 2
                P.op("pe", [("qk", 2 + j), ("qz", s)], [("ps", 5)], lambda e: e.matmul(ps[5].a(h * 128, [[1, 128]]), lhsT=qk.a((2 + j) * T + ch0, [[1, 128]]),
                                                                                   rhs=qz.a(s * 512 + j * 256 + hp_ * 128, [[1, 128]]), start=True, stop=True))
            P.op("dve", [("ps", 5), ("WT", s)], [("PT", s)], lambda e: e.tensor_tensor(out=PT.a(s * 512, [[1, 512]]), in0=ps[5].a(), in1=WT.a(s * 512, [[1, 512]]), op=ALU.mult))
            for h in range(4):
                j, hp_ = h // 2, h % 2
                hpd = dict(p0=hp_ * 64, pn=64)
                P.op("pe", ["vtok", ("PT", s)], [("ps", 6)], lambda e: e.matmul(ps[6].a((j * 2) * 128, [[1, 128]], **hpd), lhsT=vtok.a(c * 256 + h * 64, [[1, 64]]),
                                                                            rhs=PT.a(s * 512 + h * 128, [[1, 128]]), start=True, stop=False))
                P.op("pe", [("prevMb", s), ("qp", s)], [("ps", 6)], lambda e: e.matmul(ps[6].a((j * 2) * 128, [[1, 128]], **hpd), lhsT=prevMb.a(s * 256 + j * 128, [[1, 64]]),
                                                                                   rhs=qp.a(s * 512 + j * 256 + hp_ * 128, [[1, 128]]), start=False, stop=True))
                P.op("pe", ["ones_bf", ("PT", s)], [("ps", 6)], lambda e: e.matmul(ps[6].a((j * 2 + 1) * 128, [[1, 128]], **hpd), lhsT=ones_bf.a(0, [[1, 64]]),
                                                                               rhs=PT.a(s * 512 + h * 128, [[1, 128]]), start=True, stop=False))
                P.op("pe", [("prevMb", s), ("qp", s)], [("ps", 6)], lambda e: e.matmul(ps[6].a((j * 2 + 1) * 128, [[1, 128]], **hpd), lhsT=prevMb.a(s * 256 + j * 128 + 64, [[1, 64]]),
                                                                                   rhs=qp.a(s * 512 + j * 256 + hp_ * 128, [[1, 128]]), start=False, stop=True))
            P.op("dve", [("ps", 6)], ["dent"], lambda e: e.tensor_scalar(out=dent.a(0, [[128, 2], [1, 128]]), in0=ps[6].a(128, [[256, 2], [1, 128]]), scalar1=-1.0, scalar2=1.0,
                                                                       op0=ALU.mult, op1=ALU.max))
            P.op("dve", [("ps", 6), "dent"], ["dent"], lambda e: e.tensor_tensor(out=dent.a(0, [[128, 2], [1, 128]]), in0=ps[6].a(128, [[256, 2], [1, 128]]),
                                                                               in1=dent.a(0, [[128, 2], [1, 128]]), op=ALU.max))
            P.op("dve", ["dent"], ["dent"], lambda e: e.reciprocal(out=dent.a(), in_=dent.a()))
            P.op("dve", [("ps", 6), "dent"], ["hgrp"], lambda e: e.tensor_tensor(out=hgrp.a(cc * 128, [[256, 2], [1, 128]]), in0=ps[6].a(0, [[256, 2], [1, 128]]),
                                                                              in1=dent.a(0, [[128, 2], [1, 128]]), op=ALU.mult))
            if cc == 1:
                P.op("dve", ["hgrp", ("so", 0), ("so", 1)], ["hgrp"], lambda e: e.tensor_tensor(out=hgrp.a(0, [[256, 2], [1, 256]]), in0=hgrp.a(0, [[256, 2], [1, 256]]),
                                                                                           in1=so.a(g0, [[T, 2], [1, 256]]), op=ALU.mult))
                P.op("act", ["hgrp"], ["sqg"], lambda e: e.activation(out=sqg.a(0, [[1, 512]]), in_=hgrp.a(), func=AF.Square))
                for j in range(2):
                    P.op("pe", ["sqg", "onesblk_bf"], [("ps", 2, "b")], lambda e: e.matmul(ps[2].a(256, [[1, 256]]), lhsT=onesblk_bf.a(), rhs=sqg.a(j * 256, [[1, 256]]), start=True, stop=True))
                    rms_rstd(ps[2].a(256, [[1, 256]]), 64.0, rsg.a(), lng.a(), ("ps", 2, "b"), "rsg", "lng")
                    P.op("dve", ["hgrp", "rsg", ("ppt", l)], [("so", j)], lambda e: e.scalar_tensor_tensor(out=so.a(j * T + g0, [[1, 256]]), in0=hgrp.a(j * 256, [[1, 256]]),
                                                                                                        scalar=pp(l, PP_MLN + j), in1=rsg.a(), op0=ALU.mult, op1=ALU.mult))
        P.barrier()
        if l == 0:
            dbg_dump("mix_ssd", xs, BF)
            dbg_dump("mix_ml", so, BF)
        P.free(bl)
        P.free([a1, a2, a3, b2, b3])
        if stop_after == "PB":
            break

        wob, o1 = P.sb("wob", [128, KT, 1024], BF)
        for hf in range(2):
            P.dma("pool", wob.a(hf * 4 * 1024, [[1, 4 * 1024]]), wo.a(hf * 4 * 1024, [[1, 4 * 1024]], p0=l * 128, pn=128), ["wo_d"], [("wob", hf)])

        def mix_rhs(k, t0):
            if k < 2:
                return ypool.a(k * T + t0, [[1, 256]])
            if k < 6:
                return xs.a((k - 2) * T + t0, [[1, 256]])
            return so.a((k - 6) * T + t0, [[1, 256]])
        out_proj_phase(l, KT, lambda k, m: wob.a(k * 1024 + m * 128, [[1, 128]]), mix_rhs,
                       [("wob", 0), ("wob", 1), ("ypool", 0), ("ypool", 1), ("so", 0), ("so", 1)] + XSK,
                       PP_PMN, src, HALO, xres, TT, HALO, "o7_", last16=last16)
        P.free([o1])
        halo_exchange(agb + 1, last16, "last16", xres, "h2_")
        P.free(gl)
        P.free(mixer_cms)
        if stop_after == "P7":
            break

        actT, f1 = P.sb("actT", [128, NJ, T], BF)
        l16b, f1b = P.sb("l16b", [128, 128], F32)
        hT2, f2 = P.sb("hT2", [128, KT, TT], BF)
        norm_phase(l, xres, "xres_all", PP_G2, hT2, "n2_")
        cp, f3 = P.sb("cp", [128, 4, TT], F32)
        acc, f4 = P.sb("acc", [128, 4, T], F32)
        wub, f5 = P.sb("wub", [128, 2, KT, 512], BF)
        for pg in range(NJ // 2):
            ws = pg % 2
            wo_ = ws * KT * 512
            for half in range(2):
                P.dma("pool", wub.a(wo_ + half * 256, [[512, KT], [1, 256]]),
                      wu.a(half * DFF + pg * 256, [[2 * DFF, KT], [1, 256]], p0=l * 128, pn=128), ["wu_d"], [("wub", ws, half)])
            for jj in range(2):
                j = pg * 2 + jj
                sl = j % 2
                for half in range(2):
                    cb = (sl * 2 + half)
                    for (c0, n) in blocks5:
                        pi = 2 + (psrot[0] % 6)
                        psrot[0] += 1
                        for k in range(KT):
                            P.op("pe", [("wub", ws, half), "hT2"], [("ps", pi)],
                                 lambda e: e.matmul(ps[pi].a(0, [[1, n]]), lhsT=wub.a(wo_ + k * 512 + half * 256 + jj * 128, [[1, 128]]),
                                                    rhs=hT2.a(k * TT + c0, [[1, n]]), start=(k == 0), stop=(k == KT - 1)))
                        P.op("act", [("ps", pi)], [("cp", cb)],
                             lambda e: e.activation(out=cp.a(cb * TT + c0, [[1, n]]), in_=ps[pi].a(0, [[1, n]]), func=AF.Copy))
                    wcol = PP_FFN + (half * NJ + j) * 4
                    P.op("act", [("cp", cb), ("ppt", l)], [("acc", cb)],
                         lambda e: e.activation(out=acc.a(cb * T, [[1, T]]), in_=cp.a(cb * TT + HALO, [[1, T]]), func=AF.Identity,
                                                bias=pp(l, wcol + 3), scale=pp(l, wcol + 2)))
                    for tap in range(2):
                        P.op("dve", [("cp", cb), ("acc", cb), ("ppt", l)], [("acc", cb)],
                             lambda e: e.scalar_tensor_tensor(out=acc.a(cb * T, [[1, T]]), in0=cp.a(cb * TT + HALO - 2 + tap, [[1, T]]),
                                                              scalar=pp(l, wcol + tap), in1=acc.a(cb * T, [[1, T]]), op0=ALU.mult, op1=ALU.add))
                g_ = sl * 2
                P.op("act", [("acc", g_)], [("acc", g_)], lambda e: e.activation(out=acc.a(g_ * T, [[1, T]]), in_=acc.a(g_ * T, [[1, T]]), func=AF.Gelu_apprx_tanh))
                P.op("dve", [("acc", g_), ("acc", g_ + 1)], [("actT", j)], lambda e: e.tensor_tensor(out=actT.a(j * T, [[1, T]]), in0=acc.a(g_ * T, [[1, T]]), in1=acc.a((g_ + 1) * T, [[1, T]]), op=ALU.mult))
        P.barrier()
        if l == 0:
            dbg_dump("actT", actT, BF)
        P.free([f2, f3, f4, f5])
        wdb, f6 = P.sb("wdb", [128, NJ, 1024], BF)
        for hf in range(2):
            P.dma("pool", wdb.a(hf * 11 * 1024, [[1, 11 * 1024]]), wd.a(hf * 11 * 1024, [[1, 11 * 1024]], p0=l * 128, pn=128), ["wd_d"], [("wdb", hf)])
        AK = [("actT", j) for j in range(NJ)]
        if last:
            out_proj_phase(l, NJ, lambda k, m: wdb.a(k * 1024 + m * 128, [[1, 128]]), lambda k, t0: actT.a(k * T + t0, [[1, 256]]),
                           [("wdb", 0), ("wdb", 1)] + AK, PP_PFN, xres, HALO, out_d, T, 0, "o9_", last16=None)
        else:
            out_proj_phase(l, NJ, lambda k, m: wdb.a(k * 1024 + m * 128, [[1, 128]]), lambda k, t0: actT.a(k * T + t0, [[1, 256]]),
                           [("wdb", 0), ("wdb", 1)] + AK, PP_PFN, xres, HALO, xres, TT, HALO, "o9_", last16=l16b)
        P.free([f6])
        if not last:
            halo_exchange(agb + 2, l16b, "last16", xres, "h3_")
        P.free([f1, f1b])

    P.barrier()
    return P


MAIN_COLS = np.r_[0:256, 256:768, 768:1792, 1800:2312, 2568:2824]
VG_COLS = np.r_[2312:2568, 1792:1800, 2824:2828, 2828:2832]


def _kp(w, nk):
    return np.ascontiguousarray(w.reshape(nk, 128, -1).transpose(1, 0, 2))


def _consts():
    c = np.zeros((128, NCONST), np.float32)
    k = np.arange(128)
    c[:, C_U:C_U + 128] = (k[:, None] <= k[None, :]).astype(np.float32)
    c[:, C_MASK:C_MASK + 128] = np.where(k[:, None] <= k[None, :], 0.0, -30000.0)
    c[:, C_ID:C_ID + 128] = np.eye(128, dtype=np.float32)
    c[:, C_ONE:C_ONE + 128] = 1.0
    c[:, C_OBLK:C_OBLK + 128] = ((k[:, None] // 64) == (k[None, :] // 64)).astype(np.float32)
    return c


def _core_masks(c):
    b, s = c // 4, c % 4
    m = np.zeros((128, NCM), np.float32)
    if s > 0:
        m[:, CM_SEL + c - 1] = 1.0
    for j in range(NCORES):
        pre = 1.0 if (j // 4 == b and j < c) else 0.0
        m[:, CM_PREC + j] = pre
        m[:, CM_NPREC + j] = 1.0 - pre
    p = np.arange(128)
    for i in range(2):
        g = 2 * i + p // 64
        w = (2 ** (g + 1)).astype(np.float32)
        for t in range(16):
            pos = float(s * T + t + 1)
            m[:, CM_INVC + i * 16 + t] = 1.0 / np.minimum(pos, w)
    return m


def _layer_params(inp, l):
    f = lambda n: np.asarray(inp[n][l], np.float32)
    w_in = f("w_in")
    d = {}
    d["wm"] = _kp(w_in[:, MAIN_COLS], KT)
    d["wvg"] = _kp(w_in[:, VG_COLS], KT)
    d["wo"] = _kp(f("w_out"), KT)
    d["wu"] = _kp(f("ffn_w_up"), KT)
    d["wd"] = _kp(f("ffn_w_down"), NJ)
    pwl = f("pool_w")
    pwm = np.zeros((128, 2, 128), np.float32)
    for i in range(2):
        for h in range(2):
            pwm[h * 64:(h + 1) * 64, i, h * 64:(h + 1) * 64] = pwl[2 * i + h]
    d["pw"] = pwm
    pp = np.zeros((128, NPP), np.float32)
    col = lambda v, n: np.ascontiguousarray(v.reshape(n, 128).T)
    pp[:, PP_G1:PP_G1 + 8] = col(f("pre_mix_norm"), 8)
    pp[:, PP_PMN:PP_PMN + 8] = col(f("post_mix_norm"), 8)
    pp[:, PP_G2:PP_G2 + 8] = col(f("pre_ffn_norm"), 8)
    pp[:, PP_PFN:PP_PFN + 8] = col(f("post_ffn_norm"), 8)
    pp[:, PP_POOLB:PP_POOLB + 2] = col(f("pool_b"), 2)
    pp[:, PP_POOLS:PP_POOLS + 2] = col(f("pool_scale"), 2)
    p = np.arange(128)
    for i in range(2):
        pp[:, PP_INVW + i] = 1.0 / (2.0 ** (2 * i + p // 64 + 1))
    cw, cb = f("ssd_conv_w"), f("ssd_conv_b")
    for i in range(8):
        pp[:, PP_CXBC + i * 5:PP_CXBC + i * 5 + 4] = cw[:, i * 128:(i + 1) * 128].T
        pp[:, PP_CXBC + i * 5 + 4] = cb[i * 128:(i + 1) * 128]
    cw, cb = f("mlstm_conv_w"), f("mlstm_conv_b")
    for i in range(4):
        pp[:, PP_CQK + i * 5:PP_CQK + i * 5 + 4] = cw[:, i * 128:(i + 1) * 128].T
        pp[:, PP_CQK + i * 5 + 4] = cb[i * 128:(i + 1) * 128]
    sd = f("ssd_d")
    for i in range(4):
        pp[:, PP_SSDD + i] = sd[2 * i + p // 64]
    pp[:, PP_SSDN:PP_SSDN + 4] = col(f("ssd_norm"), 4)
    pp[:, PP_MLN:PP_MLN + 2] = col(f("mlstm_norm"), 2)
    cw, cb = f("ffn_conv_w"), f("ffn_conv_b")
    for i in range(2 * NJ):
        pp[:, PP_FFN + i * 4:PP_FFN + i * 4 + 3] = cw[:, i * 128:(i + 1) * 128].T
        pp[:, PP_FFN + i * 4 + 3] = cb[i * 128:(i + 1) * 128]
    d["pp"] = pp
    pb = np.zeros((128, NPB), np.float32)
    pb[:, 0:8] = f("ssd_dt_bias")[None, :]
    pb[:, 8:16] = f("ssd_a_log")[None, :]
    pb[:, 16:20] = f("mlstm_i_bias")[None, :]
    pb[:, 20:24] = f("mlstm_f_bias")[None, :]
    d["pb"] = pb
    return d


def _x_shard(x, c):
    b, s = c // 4, c % 4
    seg = np.zeros((TT, D), np.float32)
    lo = s * T - HALO
    if lo < 0:
        seg[HALO:] = x[b, 0:T]
    else:
        seg[:] = x[b, lo:lo + TT]
    return np.ascontiguousarray(seg.T.reshape(KT, 128, TT).transpose(1, 0, 2))


def _in_maps(x, inp, layers):
    lp = [_layer_params(inp, l) for l in layers]
    shared = {k: np.ascontiguousarray(np.concatenate([d[k] for d in lp], axis=0)) for k in lp[0]}
    shared["consts"] = _consts()
    maps = []
    for c in range(NCORES):
        m = dict(shared)
        m["xT"] = _x_shard(x, c)
        m["cm"] = _core_masks(c)
        maps.append(m)
    return maps


def _gather_out(results, key="out"):
    B = NCORES // 4
    out = np.zeros((B, 4 * T, D), np.float32)
    for c in range(NCORES):
        b, s = c // 4, c % 4
        o = np.asarray(results[c][key])
        out[b, s * T:(s + 1) * T, :] = o.transpose(2, 1, 0).reshape(T, D)
    return out


_CACHE = {}


def kernel(**inputs):
    x = np.asarray(inputs["x"], np.float32)
    if "prog" not in _CACHE:
        _CACHE["prog"] = build_program(DEPTH)
    P = _CACHE["prog"]
    maps = _in_maps(x, inputs, list(range(DEPTH)))
    res = run_bass_kernel_spmd(P.nc, maps, core_ids=list(range(NCORES)))
    return _gather_out(res.results)
```
